# Optimizing a Trainium2 kernel written in Bass

```python
import jax
import jax.numpy as jnp
from jax import lax
import numpy as np

D_MODEL = 1024
BATCH = 4
SEQ = 8192
DEPTH = 2

CHUNK = 64
Q_BLOCK = 128
HEAD_DIM = 64
H_SPARSE = 8
H_FOX = 8
H_IDX = 8
D_IDX = 64
TOPK_MAX = 256
ROPE_THETA = 500000.0
ROT_DIM = HEAD_DIM // 4
ROT_DIM_IDX = D_IDX // 4
D_FF = 2816
CONV_W = 3
N_BRANCH = 2
EPS = 1e-6
NEG_INF = -1e30

W_SPARSE = H_SPARSE * HEAD_DIM
W_FOX = H_FOX * HEAD_DIM
IN_SPLITS = (W_SPARSE, HEAD_DIM, HEAD_DIM, H_IDX * D_IDX, D_IDX, H_IDX,
             W_FOX, W_FOX, W_FOX, H_FOX, N_BRANCH * D_MODEL)
N_IN = W_SPARSE + 2 * HEAD_DIM + H_IDX * D_IDX + D_IDX + H_IDX + 3 * W_FOX + H_FOX + N_BRANCH * D_MODEL

kernel_name = "hybrid_dsa_fox_convffn_adaln"


def _rmsnorm(x, g):
    xf = x.astype(jnp.float32)
    y = xf * lax.rsqrt(jnp.mean(xf * xf, axis=-1, keepdims=True) + EPS)
    return (y * g.astype(jnp.float32)).astype(x.dtype)


def _partial_rope(x, positions, rot_dim):
    half = rot_dim // 2
    inv_freq = ROPE_THETA ** (-jnp.arange(0, rot_dim, 2, dtype=jnp.float32) / rot_dim)
    ang = positions.astype(jnp.float32)[..., None] * inv_freq
    cos = jnp.cos(ang)[:, :, None, :]
    sin = jnp.sin(ang)[:, :, None, :]
    xf = x.astype(jnp.float32)
    x1, x2, rest = xf[..., :half], xf[..., half:rot_dim], xf[..., rot_dim:]
    out = jnp.concatenate([x1 * cos - x2 * sin, x2 * cos + x1 * sin, rest], axis=-1)
    return out.astype(x.dtype)


def _to_blocks(a, nb):
    b = a.shape[0]
    return jnp.moveaxis(a.reshape((b, nb, Q_BLOCK) + a.shape[2:]), 1, 0)


def _from_blocks(a):
    a = jnp.moveaxis(a, 0, 1)
    return a.reshape((a.shape[0], a.shape[1] * a.shape[2]) + a.shape[3:])


def _sparse_attention(q, k, v, q_idx, k_idx, w_idx):
    S = q.shape[1]
    nb = S // Q_BLOCK
    topk = min(TOPK_MAX, S // 4)
    key_chunk = jnp.arange(S) // CHUNK
    idx_scale = D_IDX ** -0.5
    att_scale = HEAD_DIM ** -0.5

    def block(args):
        qb, qib, wb, q_start = args
        q_chunk = (q_start + jnp.arange(Q_BLOCK)) // CHUNK
        admissible = key_chunk[None, :] <= q_chunk[:, None]
        s = jnp.einsum("bqhd,bsd->bqhs", qib, k_idx).astype(jnp.float32) * idx_scale
        score = jnp.einsum("bqh,bqhs->bqs", wb.astype(jnp.float32), jax.nn.relu(s))
        score = jnp.where(admissible[None], score, NEG_INF)
        top_val, top_idx = lax.top_k(score, topk)
        valid = top_val > 0.5 * NEG_INF
        k_sel = jax.vmap(lambda kk, ii: kk[ii])(k, top_idx)
        v_sel = jax.vmap(lambda vv, ii: vv[ii])(v, top_idx)
        logits = jnp.einsum("bqhd,bqkd->bqhk", qb, k_sel).astype(jnp.float32) * att_scale
        logits = jnp.where(valid[:, :, None, :], logits, NEG_INF)
        p = jax.nn.softmax(logits, axis=-1).astype(v.dtype)
        return jnp.einsum("bqhk,bqkd->bqhd", p, v_sel)

    starts = jnp.arange(nb) * Q_BLOCK
    out = lax.map(block, (_to_blocks(q, nb), _to_blocks(q_idx, nb), _to_blocks(w_idx, nb), starts))
    return _from_blocks(out)


def _forgetting_attention(q, k, v, log_f):
    S = q.shape[1]
    nb = S // Q_BLOCK
    cum = jnp.cumsum(log_f, axis=1)
    cum_k = jnp.moveaxis(cum, 1, 2)
    key_pos = jnp.arange(S)
    scale = HEAD_DIM ** -0.5

    def block(args):
        qb, cq, q_start = args
        causal = key_pos[None, :] <= (q_start + jnp.arange(Q_BLOCK))[:, None]
        logits = jnp.einsum("bqhd,bshd->bhqs", qb, k).astype(jnp.float32) * scale
        logits = logits + jnp.moveaxis(cq, 1, 2)[..., None] - cum_k[:, :, None, :]
        logits = jnp.where(causal[None, None], logits, NEG_INF)
        p = jax.nn.softmax(logits, axis=-1).astype(v.dtype)
        return jnp.einsum("bhqs,bshd->bqhd", p, v)

    starts = jnp.arange(nb) * Q_BLOCK
    out = lax.map(block, (_to_blocks(q, nb), _to_blocks(cum, nb), starts))
    return _from_blocks(out)


def _mixer(h, positions, w_in, forget_bias, w_branch_a, w_branch_b, w_out):
    B, S, _ = h.shape
    proj = h @ w_in
    offs = np.cumsum(IN_SPLITS)[:-1].tolist()
    q_a, k_a, v_a, q_i, k_i, w_i, q_f, k_f, v_f, f_f, gates = jnp.split(proj, offs, axis=-1)
    q_a = _partial_rope(q_a.reshape(B, S, H_SPARSE, HEAD_DIM), positions, ROT_DIM)
    k_a = _partial_rope(k_a[:, :, None, :], positions, ROT_DIM)[:, :, 0]
    q_i = _partial_rope(q_i.reshape(B, S, H_IDX, D_IDX), positions, ROT_DIM_IDX)
    k_i = _partial_rope(k_i[:, :, None, :], positions, ROT_DIM_IDX)[:, :, 0]
    w_i = w_i * (H_IDX ** -0.5)
    o_a = _sparse_attention(q_a, k_a, v_a, q_i, k_i, w_i)
    y_a = o_a.reshape(B, S, W_SPARSE) @ w_branch_a
    log_f = jax.nn.log_sigmoid(f_f.astype(jnp.float32) + forget_bias.astype(jnp.float32))
    o_b = _forgetting_attention(q_f.reshape(B, S, H_FOX, HEAD_DIM),
                                k_f.reshape(B, S, H_FOX, HEAD_DIM),
                                v_f.reshape(B, S, H_FOX, HEAD_DIM), log_f)
    y_b = o_b.reshape(B, S, W_FOX) @ w_branch_b
    g_a, g_b = jnp.split(jax.nn.sigmoid(gates.astype(jnp.float32)).astype(h.dtype), N_BRANCH, axis=-1)
    return (g_a * y_a + g_b * y_b) @ w_out


def _conv_ffn(h, w_up, conv_w, conv_b, w_down):
    S = h.shape[1]
    u = h @ w_up
    up = jnp.pad(u, ((0, 0), (CONV_W - 1, 0), (0, 0)))
    u = conv_b + sum(conv_w[i] * up[:, i:i + S] for i in range(CONV_W))
    a, b = jnp.split(u, 2, axis=-1)
    return (jax.nn.silu(a) * b) @ w_down


def setup_inputs(seed: int = 0) -> dict:
    key = jax.random.key(seed)
    ks = jax.random.split(key, 17)
    f32 = jnp.float32

    def nrm(k, shape, fan_in, mult=1.0):
        return jax.random.normal(k, shape, f32) * (mult * fan_in ** -0.5)

    x = jax.random.normal(ks[0], (BATCH, SEQ, D_MODEL), f32)
    c = jax.random.normal(ks[1], (BATCH, D_MODEL), f32)
    offset = jax.random.randint(ks[2], (BATCH, 1), 0, 4096, dtype=jnp.int32)
    positions = offset + jnp.arange(SEQ, dtype=jnp.int32)[None, :]
    mod_w = nrm(ks[3], (DEPTH, D_MODEL, 6 * D_MODEL), D_MODEL, 0.5)
    mod_b = 0.01 * jax.random.normal(ks[4], (DEPTH, 6 * D_MODEL), f32)
    norm1_g = 1.0 + 0.02 * jax.random.normal(ks[5], (DEPTH, D_MODEL), f32)
    norm2_g = 1.0 + 0.02 * jax.random.normal(ks[6], (DEPTH, D_MODEL), f32)
    w_in = nrm(ks[7], (DEPTH, D_MODEL, N_IN), D_MODEL)
    forget_bias = 4.0 + 0.5 * jax.random.normal(ks[8], (DEPTH, H_FOX), f32)
    w_branch_a = nrm(ks[9], (DEPTH, W_SPARSE, D_MODEL), W_SPARSE)
    w_branch_b = nrm(ks[10], (DEPTH, W_FOX, D_MODEL), W_FOX)
    w_out = nrm(ks[11], (DEPTH, D_MODEL, D_MODEL), D_MODEL)
    w_up = nrm(ks[12], (DEPTH, D_MODEL, 2 * D_FF), D_MODEL)
    conv_w = nrm(ks[13], (DEPTH, CONV_W, 2 * D_FF), CONV_W)
    conv_b = 0.01 * jax.random.normal(ks[14], (DEPTH, 2 * D_FF), f32)
    w_down = nrm(ks[15], (DEPTH, D_FF, D_MODEL), D_FF)
    final_g = 1.0 + 0.02 * jax.random.normal(ks[16], (D_MODEL,), f32)
    return {"x": x, "c": c, "positions": positions, "mod_w": mod_w, "mod_b": mod_b,
            "norm1_g": norm1_g, "norm2_g": norm2_g, "w_in": w_in, "forget_bias": forget_bias,
            "w_branch_a": w_branch_a, "w_branch_b": w_branch_b, "w_out": w_out,
            "w_up": w_up, "conv_w": conv_w, "conv_b": conv_b, "w_down": w_down,
            "final_g": final_g}


def reference(x, c, positions, mod_w, mod_b, norm1_g, norm2_g, w_in, forget_bias,
              w_branch_a, w_branch_b, w_out, w_up, conv_w, conv_b, w_down, final_g):
    for l in range(DEPTH):
        mod = c @ mod_w[l] + mod_b[l]
        sh1, sc1, g1, sh2, sc2, g2 = [m[:, None, :] for m in jnp.split(mod, 6, axis=-1)]
        h = _rmsnorm(x, norm1_g[l]) * (1.0 + sc1) + sh1
        x = x + g1 * _mixer(h, positions, w_in[l], forget_bias[l], w_branch_a[l], w_branch_b[l], w_out[l])
        h = _rmsnorm(x, norm2_g[l]) * (1.0 + sc2) + sh2
        x = x + g2 * _conv_ffn(h, w_up[l], conv_w[l], conv_b[l], w_down[l])
    return _rmsnorm(x, final_g)
```

```python
import contextlib
import numpy as np
import concourse.bass as bass
import concourse.mybir as mybir
from concourse.bass_utils import run_bass_kernel_spmd

F32 = mybir.dt.float32
BF16 = mybir.dt.bfloat16
I32 = mybir.dt.int32
ALU = mybir.AluOpType
AF = mybir.ActivationFunctionType
AX = mybir.AxisListType

D = 1024
S = 8192
NB = 4
DEPTH = 2
NT = 32
SL = NT * 128
NGT = 64
HD = 64
NH = 8
DFF = 2816
NFF = DFF // 128
N_IN = 4816
TOPK = 256
EPS = 1e-6
NEG = -1.0e30
ROPE_THETA = 500000.0
N_BISECT = 22

DEBUG = {}

O_QA, O_KA, O_VA, O_QI, O_KI, O_WI = 0, 512, 576, 640, 1152, 1216
O_QF, O_KF, O_VF, O_FF, O_G = 1224, 1736, 2248, 2760, 2768
SB_QA, SB_KA, SB_QI, SB_KI, SB_QF, SB_KF, SB_VF, SB_FF, SB_G, NW = 0, 512, 640, 1152, 1280, 1792, 2304, 2816, 2880, 4928


class Op:
    __slots__ = ("eng", "fn", "deps", "is_dma", "sem", "val", "sig", "need_sig")

    def __init__(self, eng, fn, is_dma):
        self.eng = eng
        self.fn = fn
        self.deps = []
        self.is_dma = is_dma
        self.sem = None
        self.val = 0
        self.sig = 0
        self.need_sig = False


class _Rec:
    def __init__(self):
        self.call = None

    def __getattr__(self, name):
        def f(*a, **k):
            assert self.call is None
            self.call = (name, a, k)
            return None
        return f


def _freeze(fn):
    rec = _Rec()
    fn(rec)
    c = rec.call
    assert c is not None
    return lambda e, c=c: getattr(e, c[0])(*c[1], **c[2])


class Prog:
    ENGINES = ("pe", "act", "dve", "pool", "sp")
    NDMA = {"sp": 16, "pool": 16, "act": 4}
    EPOCH = 4000

    def __init__(self):
        self.ops = []
        self.last_w = {}
        self.readers = {}
        self.dma_rr = {q: 0 for q in self.NDMA}
        self.dma_cnt = {}
        self.dma_last = {}
        self.last_compute = {}
        self.pending = {e: None for e in self.ENGINES}
        self.sticky = set()
        self.wcc_n = 0

    def _add(self, op, reads, writes):
        deps = op.deps
        pb = self.pending[op.eng]
        if pb is not None:
            deps.extend(pb)
            self.pending[op.eng] = None
        for k in reads:
            w = self.last_w.get(k)
            if w is not None:
                deps.append(w)
        for k in writes:
            w = self.last_w.get(k)
            if w is not None:
                deps.append(w)
            rs = self.readers.get(k)
            if rs:
                deps.extend(rs)
        for k in reads:
            self.readers.setdefault(k, []).append(op)
        for k in writes:
            self.last_w[k] = op
            self.readers[k] = []
        self.ops.append(op)
        return op

    def op(self, eng, fn, reads=(), writes=()):
        o = Op(eng, _freeze(fn), False)
        self._add(o, reads, writes)
        self.last_compute[eng] = o
        return o

    def dma(self, q, out, in_, reads=(), writes=(), **kw):
        o = Op(q, (lambda e, out=out, in_=in_, kw=kw: e.dma_start(out=out, in_=in_, **kw)), True)
        k = self.dma_rr[q] % self.NDMA[q]
        self.dma_rr[q] += 1
        key = (q, k)
        prev = self.dma_last.get(key)
        if prev is not None:
            o.deps.append(prev)
        self.dma_cnt[key] = self.dma_cnt.get(key, 0) + 1
        o.sem = key
        o.val = 16 * self.dma_cnt[key]
        self.dma_last[key] = o
        return self._add(o, reads, writes)

    def cc(self, fn, reads=(), writes=(), weight=False):
        o = Op("pool", _freeze(fn), True)
        if weight:
            key = ("wcc", self.wcc_n)
            self.wcc_n += 1
            self.sticky.update(writes)
        else:
            key = ("cc", 0)
        prev = self.dma_last.get(key)
        if prev is not None:
            o.deps.append(prev)
        self.dma_cnt[key] = self.dma_cnt.get(key, 0) + 1
        o.sem = key
        o.val = self.dma_cnt[key]
        self.dma_last[key] = o
        return self._add(o, reads, writes)

    def barrier(self):
        fr = list(self.last_compute.values()) + [o for k, o in self.dma_last.items() if k[0] != "wcc"]
        for e in self.ENGINES:
            if self.pending[e] is None:
                self.pending[e] = list(fr)
            else:
                self.pending[e] = self.pending[e] + fr
        self.last_w = {k: v for k, v in self.last_w.items() if k in self.sticky}
        self.readers = {}

    def emit(self, nc, stack):
        for o in self.ops:
            for d in o.deps:
                if d.is_dma:
                    continue
                if d.eng == "pe" and o.eng == "pe" and not o.is_dma:
                    continue
                d.need_sig = True
        EPOCH = self.EPOCH
        cnt = {e: 0 for e in self.ENGINES}
        for o in self.ops:
            if (not o.is_dma) and o.need_sig:
                cnt[o.eng] += 1
                o.sig = cnt[o.eng]
        sems = {}
        for e in self.ENGINES:
            for ep in range((cnt[e] + EPOCH - 1) // EPOCH + 1):
                sems[("c", e, ep)] = stack.enter_context(nc.semaphore("s_%s%d" % (e, ep)))
        for key in self.dma_cnt:
            sems[key] = stack.enter_context(nc.semaphore("d_%s%d" % key))
        by_eng = {e: [] for e in self.ENGINES}
        for o in self.ops:
            by_eng[o.eng].append(o)
        final = list(self.last_compute.values()) + list(self.dma_last.values())

        def run(ename, eng):
            waited = {}

            def wait_for(d, consumer_is_pe_compute):
                if d.is_dma:
                    key, val, gval = d.sem, d.val, d.val
                    wkey = key
                else:
                    if d.eng == "pe" and consumer_is_pe_compute:
                        return
                    assert d.sig > 0
                    ep = (d.sig - 1) // EPOCH
                    key, val, gval = ("c", d.eng, ep), d.sig - ep * EPOCH, d.sig
                    wkey = ("c", d.eng)
                if waited.get(wkey, 0) >= gval:
                    return
                waited[wkey] = gval
                eng.wait_ge(sems[key], val)

            for o in by_eng[ename]:
                for d in o.deps:
                    wait_for(d, ename == "pe" and not o.is_dma)
                ins = o.fn(eng)
                if o.is_dma:
                    ins.then_inc(sems[o.sem], 1 if o.sem[0] in ("cc", "wcc") else 16)
                elif o.need_sig:
                    ins.then_inc(sems[("c", o.eng, (o.sig - 1) // EPOCH)], 1)
            if ename == "sp":
                for d in final:
                    if d.is_dma or d.need_sig:
                        wait_for(d, False)

        with nc.Block() as block:
            @block.tensor
            def _(e):
                run("pe", e)

            @block.scalar
            def _(e):
                run("act", e)

            @block.vector
            def _(e):
                run("dve", e)

            @block.gpsimd
            def _(e):
                run("pool", e)

            @block.sync
            def _(e):
                run("sp", e)


def configure(seq):
    global S, NT, SL, NGT
    S = seq
    NGT = S // 128
    NT = NGT // 2
    SL = NT * 128


class Builder:
    def __init__(self, dbg=None, ncores=8):
        self.dbg = dbg or {}
        self.ncores = ncores
        self.RG = [[2 * i, 2 * i + 1] for i in range(ncores // 2)]
        self.RGALL = [list(range(ncores))]
        self.nbatch = ncores // 2
        self.nc = bass.Bass("TRN2", target_bir_lowering=False)
        self.P = Prog()
        self.stack = contextlib.ExitStack()
        self.uid = 0

    def din(self, name, shape, dt=F32):
        return self.nc.dram_tensor(name, list(shape), dt, kind="ExternalInput").ap()

    def dout(self, name, shape, dt=F32):
        return self.nc.dram_tensor(name, list(shape), dt, kind="ExternalOutput").ap()

    def dscr(self, name, shape, dt=F32):
        return self.nc.dram_tensor(name, list(shape), dt).ap()

    def sb(self, name, shape, dt=F32):
        return self.stack.enter_context(self.nc.sbuf_tensor(name, list(shape), dt))

    def build(self):
        nc, P = self.nc, self.P
        stop = self.dbg.get("stop_after")
        with self.stack:
            self.declare()
            self.prologue()
            done = (stop == "P")
            if done:
                self.dump("modv")
            for l in range(DEPTH):
                if done:
                    break
                for nm, fn in (("A", self.stage_a), ("G", self.stage_gather), ("B", self.stage_b),
                               ("S", self.stage_s), ("H", self.stage_halo), ("D", self.stage_d)):
                    fn(l)
                    if stop == "%s%d" % (nm, l):
                        done = True
                        break
            if done:
                for nm in self.dbg.get("dump", []):
                    self.dump(nm)
            P.barrier()
            P.emit(nc, self.stack)
        return nc

    def declare(self):
        d = self
        d.x_in = d.din("x", [SL, D])
        d.pos = d.din("pos", [128, NT], I32)
        d.invf = d.din("invf", [128, 8])
        d.ident_in = d.din("ident", [128, 128])
        d.tri_in = d.din("tri", [128, 128])
        d.su2_in = d.din("su2", [NGT, NGT])
        d.fm_in = d.din("fmask", [2, 128, 128])
        d.madd_in = d.din("madd", [2, 128, 128])
        d.sel_in = d.din("selcol", [128, 2])
        d.hmask_in = d.din("hmask", [128, 2 * NT])
        NC_ = d.ncores
        d.CW = 6 * D // NC_
        d.mod_w_sh = d.din("mod_w_sh", [DEPTH, D, d.CW])
        d.mod_b_sh = d.din("mod_b_sh", [DEPTH, d.CW])
        d.cT_all = d.din("cT_all", [128, 8, d.nbatch])
        d.bsel = d.din("bsel", [1, d.nbatch])
        d.MODSH = d.dscr("MODSH", [DEPTH * d.nbatch, d.CW])
        d.MODG = d.dscr("MODG", [NC_, DEPTH * d.nbatch, d.CW])
        d.n1g = d.din("n1g", [DEPTH, 128, 8])
        d.n2g = d.din("n2g", [DEPTH, 128, 8])
        d.fbias = d.din("fbias", [DEPTH, NH])

        def wsh(name, rows, cols):
            if name not in d.dbg.get("wlist", ("w_in", "w_a", "w_b", "w_o", "w_up", "w_dn")):
                return None
            sh = d.din(name + "_sh", [rows // NC_, cols])
            sc = d.dscr(name + "_sc", [rows // NC_, cols])
            full = d.dscr(name + "_full", [rows, cols])
            d.wlist.append((name, sh, sc, full))
            return full.rearrange("(l r) n -> l r n", l=DEPTH)

        d.wlist = []
        d.w_in = wsh("w_in", DEPTH * D, N_IN)
        d.w_a = wsh("w_a", DEPTH * 512, D)
        d.w_b = wsh("w_b", DEPTH * 512, D)
        d.w_o = wsh("w_o", DEPTH * D, D)
        d.w_up = wsh("w_up", DEPTH * D, 2 * DFF)
        d.w_dn = wsh("w_dn", DEPTH * DFF, D)
        d.convw = d.din("convw", [DEPTH, 128, 3, 2 * NFF])
        d.convb = d.din("convb", [DEPTH, 128, 2 * NFF])
        d.fgain = d.din("fgain", [1, D])
        d.out = d.dout("out", [SL, D])
        d.modv = d.dscr("modv", [DEPTH, 6 * D])
        d.XRES = d.dscr("XRES", [SL, D])
        d.QFT = d.dscr("QFT", [4, 128, SL], BF16)
        d.QAT = d.dscr("QAT", [4, 128, SL], BF16)
        d.QIT = d.dscr("QIT", [4, 128, SL], BF16)
        d.GT = d.dscr("GT", [16, 128, SL], BF16)
        d.WI = d.dscr("WI", [SL, NH])
        d.OBT = d.dscr("OBT", [64, NH, SL], BF16)
        d.OFF = d.dscr("OFF", [NGT, NH])
        d.HALO = d.dscr("HALO", [2 * NT, D])
        d.KFT = d.dscr("KFT", [4, 128, SL], BF16)
        d.VF = d.dscr("VF", [SL, 512], BF16)
        d.KAT2 = d.dscr("KAT2", [128, SL], BF16)
        d.KIT2 = d.dscr("KIT2", [128, SL], BF16)
        d.VA = d.dscr("VA", [SL, HD], BF16)
        d.CUML = d.dscr("CUML", [128, NT * NH])
        d.CHV = min(4, NT)
        d.G_KFT_p = [d.dscr("G_KFT%d" % i, [2, 64, SL], BF16) for i in range(8)]
        d.G_VF_p = [d.dscr("G_VF%d" % i, [2, d.CHV * 128, 512], BF16) for i in range(NT // d.CHV)]
        d.G_KAT2_p = [d.dscr("G_KAT2_%d" % i, [2, 64, SL], BF16) for i in range(2)]
        d.G_KIT2_p = [d.dscr("G_KIT2_%d" % i, [2, 64, SL], BF16) for i in range(2)]
        d.G_KAT2 = d.dscr("G_KAT2", [2, 128, SL], BF16)
        d.G_KIT2 = d.dscr("G_KIT2", [2, 128, SL], BF16)
        d.G_VA = d.dscr("G_VA", [2, SL, HD], BF16)
        d.G_CUML = d.dscr("G_CUML", [2, 128, NT * NH])
        d.G_HALO = d.dscr("G_HALO", [2, 2 * NT, D])
        d.ARENA = 198 * 1024
        d.arena = d.sb("arena", [128, d.ARENA // 2], BF16)
        d.aoff = 0
        d.ident_f = d.sb("ident_f", [128, 128])
        d.ident_b = d.sb("ident_b", [128, 128], BF16)
        d.tri_f = d.sb("tri_f", [128, 128])
        d.ones_b = d.sb("ones_b", [128, 64], BF16)
        d.fmask = d.sb("fmask_sb", [128, 2, 128])
        d.madd = d.sb("madd_sb", [128, 2, 128])
        d.selc = d.sb("selc", [128, 2])
        d.cs = d.sb("cs", [128, NT, 8])
        d.sn = d.sb("sn", [128, NT, 8])
        d.ps = [self.stack.enter_context(self.nc.psum_tensor("ps%d" % i, [128, 512], F32)) for i in range(8)]

    def arena_reset(self):
        self.aoff = 0

    def carve(self, shape, dt=BF16):
        n = 1
        for s in shape:
            n *= s
        nbytes = n * (4 if dt in (F32, I32) else 2)
        nbytes = (nbytes + 127) // 128 * 128
        assert self.aoff + nbytes <= self.ARENA, "arena overflow %d" % (self.aoff + nbytes)
        v = self.arena[:, self.aoff // 2:(self.aoff + nbytes) // 2]
        self.aoff += nbytes
        if dt in (F32, I32):
            v = v.bitcast(dt)
        v = v[:, 0:n]
        if len(shape) == 2:
            return v.rearrange("p (a b) -> p a b", a=shape[0], b=shape[1])
        if len(shape) == 3:
            return v.rearrange("p (a b c) -> p a b c", a=shape[0], b=shape[1], c=shape[2])
        return v

    def dump(self, nm):
        src = getattr(self, nm)
        out = self.nc.dram_tensor("dbg_" + nm, list(src.shape), src.dtype, kind="ExternalOutput").ap()
        self.P.dma("sp", out, src, writes=[self.key("dbg")])

    def key(self, base):
        self.uid += 1
        return "%s#%d" % (base, self.uid)

    def prologue(self):
        d, P = self, self.P
        P.dma("sp", d.ident_f[:], d.ident_in[:, :], writes=["ident_f"])
        P.dma("sp", d.tri_f[:], d.tri_in[:, :], writes=["tri_f"])
        P.dma("sp", d.fmask[:], d.fm_in.rearrange("a p q -> p a q"), writes=["fmask"])
        P.dma("sp", d.madd[:], d.madd_in.rearrange("a p q -> p a q"), writes=["madd"])
        P.dma("sp", d.selc[:], d.sel_in[:, :], writes=["selc"])
        P.op("dve", lambda e: e.tensor_copy(out=d.ident_b[:], in_=d.ident_f[:]), reads=["ident_f"], writes=["ident_b"])
        P.op("dve", lambda e: e.memset(d.ones_b[:], 1.0), writes=["ones_b"])
        posi = d.carve([NT], I32)
        posf = d.carve([NT], F32)
        invf = d.carve([8], F32)
        ang = d.carve([NT, 8], F32)
        kf = d.carve([NT, 8], F32)
        ki = d.carve([NT, 8], I32)
        m = d.carve([NT, 8], F32)
        P.dma("sp", posi, d.pos[:, :], writes=["posi"])
        P.dma("sp", invf, d.invf[:, :], writes=["invf"])
        P.op("dve", lambda e: e.tensor_copy(out=posf, in_=posi), reads=["posi"], writes=["posf"])
        P.op("dve", lambda e: e.tensor_tensor(out=ang, in0=posf.unsqueeze(2).to_broadcast([128, NT, 8]),
                                              in1=invf.unsqueeze(1).to_broadcast([128, NT, 8]), op=ALU.mult),
             reads=["posf", "invf"], writes=["ang"])
        TWO_PI = float(2 * np.pi)

        def reduce_into(dst, shift, tag):
            P.op("dve", lambda e: e.tensor_scalar(out=kf, in0=ang, scalar1=1.0 / TWO_PI, scalar2=shift / TWO_PI + 0.5,
                                                  op0=ALU.mult, op1=ALU.add), reads=["ang", "ki"], writes=["kf"])
            P.op("dve", lambda e: e.tensor_copy(out=ki, in_=kf), reads=["kf"], writes=["ki"])
            P.op("dve", lambda e: e.tensor_copy(out=kf, in_=ki), reads=["ki"], writes=["kf"])
            P.op("dve", lambda e: e.tensor_scalar(out=kf, in0=kf, scalar1=-TWO_PI, scalar2=shift, op0=ALU.mult, op1=ALU.add),
                 reads=["kf"], writes=["kf"])
            P.op("dve", lambda e: e.tensor_tensor(out=dst, in0=kf, in1=ang, op=ALU.add), reads=["kf", "ang"], writes=[tag])
            P.op("dve", lambda e: e.tensor_single_scalar(out=m, in_=dst, scalar=-float(np.pi), op=ALU.is_lt), reads=[tag], writes=["m"])
            P.op("dve", lambda e: e.scalar_tensor_tensor(out=dst, in0=m, scalar=TWO_PI, in1=dst, op0=ALU.mult, op1=ALU.add),
                 reads=["m", tag], writes=[tag])
            P.op("dve", lambda e: e.tensor_single_scalar(out=m, in_=dst, scalar=float(np.pi), op=ALU.is_gt), reads=[tag], writes=["m"])
            P.op("dve", lambda e: e.scalar_tensor_tensor(out=dst, in0=m, scalar=-TWO_PI, in1=dst, op0=ALU.mult, op1=ALU.add),
                 reads=["m", tag], writes=[tag])
            P.op("dve", lambda e: e.tensor_scalar(out=dst, in0=dst, scalar1=3.1415925, scalar2=-3.1415925, op0=ALU.min, op1=ALU.max),
                 reads=[tag], writes=[tag])

        reduce_into(d.sn[:], 0.0, "sn")
        reduce_into(d.cs[:], float(np.pi / 2), "cs")
        P.op("act", lambda e: e.activation(out=d.sn[:], in_=d.sn[:], func=AF.Sin), reads=["sn"], writes=["sn"])
        P.op("act", lambda e: e.activation(out=d.cs[:], in_=d.cs[:], func=AF.Sin), reads=["cs"], writes=["cs"])

        for (name, sh, sc, full) in d.wlist:
            P.dma("pool", sc, sh, writes=["wsc_" + name])
            rg = d.RGALL
            P.cc(lambda e, sc=sc, full=full: e.collective_compute("AllGather", ALU.bypass, replica_groups=rg, ins=[sc], outs=[full]),
                 reads=["wsc_" + name], writes=[name], weight=True)
        NBt, CW = d.nbatch, d.CW
        CK = 512 if CW % 512 == 0 else 384
        cTa = d.carve([8, NBt], F32)
        bsel = d.carve([NBt], F32)
        wch = d.carve([8, CW], F32)
        modb = d.carve([CW], F32)
        msh = d.carve([CW], F32)
        P.dma("sp", cTa, d.cT_all[:, :, :], writes=["cTa"])
        P.dma("sp", bsel[0:1, :], d.bsel[:, :], writes=["bsel"])
        for l in range(DEPTH):
            P.dma("sp", wch, d.mod_w_sh[l].rearrange("(kc p) n -> p kc n", p=128), writes=["wch"])
            P.dma("sp", modb[0:NBt, :], d.mod_b_sh[l:l + 1, :].partition_broadcast(NBt), writes=["modb"])
            for c0 in range(0, CW, CK):
                for kc in range(8):
                    P.op("pe", lambda e, kc=kc, c0=c0: e.matmul(d.ps[0][0:NBt, 0:CK], lhsT=cTa[:, kc, :], rhs=wch[:, kc, c0:c0 + CK],
                                                               start=(kc == 0), stop=(kc == 7)), reads=["cTa", "wch"], writes=["ps0"])
                P.op("dve", lambda e, c0=c0: e.tensor_tensor(out=msh[0:NBt, c0:c0 + CK], in0=d.ps[0][0:NBt, 0:CK], in1=modb[0:NBt, c0:c0 + CK],
                                                            op=ALU.add), reads=["ps0", "modb"], writes=["msh"])
            P.dma("sp", d.MODSH[l * NBt:(l + 1) * NBt, :], msh[0:NBt, :], reads=["msh"], writes=["MODSH"])
        rg = d.RGALL
        P.cc(lambda e: e.collective_compute("AllGather", ALU.bypass, replica_groups=rg, ins=[d.MODSH],
                                            outs=[d.MODG.rearrange("k r c -> (k r) c")]), reads=["MODSH"], writes=["MODG"])
        modrow = d.carve([6 * D], F32)
        cand = d.carve([6 * D], F32)
        for l in range(DEPTH):
            for b in range(NBt):
                row = l * NBt + b
                P.dma("sp", cand[0:1, :].rearrange("p (k c) -> p k c", c=CW), d.MODG[:, row:row + 1, :].rearrange("k o c -> o k c"),
                      reads=["MODG"], writes=["cand"])
                if b == 0:
                    P.op("dve", lambda e, b=b: e.tensor_scalar(out=modrow[0:1, :], in0=cand[0:1, :], scalar1=bsel[0:1, b:b + 1], scalar2=None,
                                                               op0=ALU.mult), reads=["cand", "bsel"], writes=["modrow"])
                else:
                    P.op("dve", lambda e, b=b: e.scalar_tensor_tensor(out=modrow[0:1, :], in0=cand[0:1, :], scalar=bsel[0:1, b:b + 1],
                                                                      in1=modrow[0:1, :], op0=ALU.mult, op1=ALU.add),
                         reads=["cand", "bsel", "modrow"], writes=["modrow"])
            P.dma("sp", d.modv[l:l + 1, :], modrow[0:1, :], reads=["modrow"], writes=["modv%d" % l])
        P.barrier()
        d.arena_reset()

    def mod_cols(self, l):
        d, P = self, self.P
        cols = d.carve([6, 8], F32)
        mrow = d.carve([128], F32)
        P.dma("sp", mrow[0:48, :], d.modv[l].rearrange("(j p) -> j p", p=128), writes=["mrow"])
        P.op("pe", lambda e: e.transpose(out=d.ps[0][:, 0:48], in_=mrow[0:48, :], identity=d.ident_f[0:48, 0:48]),
             reads=["mrow", "ident_f"], writes=["ps0"])
        P.op("dve", lambda e: e.tensor_copy(out=cols, in_=d.ps[0][:, 0:48].rearrange("p (w kc) -> p w kc", kc=8)),
             reads=["ps0"], writes=["cols"])
        return cols

    def norm_setup(self, l, cols, which, gain_dram):
        d, P = self, self.P
        ng = d.carve([8], F32)
        A = d.carve([8], F32)
        P.dma("sp", ng, gain_dram[l], writes=["ng"])
        P.op("dve", lambda e: e.tensor_scalar(out=A, in0=cols[:, which + 1, :], scalar1=1.0, scalar2=None, op0=ALU.add),
             reads=["cols"], writes=["A"])
        P.op("dve", lambda e: e.tensor_tensor(out=A, in0=A, in1=ng, op=ALU.mult), reads=["A", "ng"], writes=["A"])
        return A, cols[:, which, :]

    def norm_bufs(self):
        d = self
        return dict(xt=[d.carve([D], F32) for _ in range(2)],
                    xn=[d.carve([D], BF16) for _ in range(2)], st=[d.carve([4], F32) for _ in range(2)])

    def norm_tile(self, nb, s, rows, x_src_ap, xkey, A, sh, hT_dst, load=True):
        d, P = self, self.P
        xt, xn, st = nb["xt"][s], nb["xn"][s], nb["st"][s]
        if load:
            P.dma("sp", xt[0:rows, :], x_src_ap, reads=[xkey], writes=["xt%d" % s])
        P.op("act", lambda e: e.activation(out=xn[0:rows, :], in_=xt[0:rows, :], func=AF.Square, accum_out=st[0:rows, 0:1]),
             reads=["xt%d" % s], writes=["xn%d" % s, "st%d" % s])
        P.op("act", lambda e: e.activation(out=st[0:rows, 1:2], in_=st[0:rows, 0:1], func=AF.Ln, scale=1.0 / D, bias=EPS),
             reads=["st%d" % s], writes=["st%d" % s])
        P.op("act", lambda e: e.activation(out=st[0:rows, 2:3], in_=st[0:rows, 1:2], func=AF.Exp, scale=-0.5),
             reads=["st%d" % s], writes=["st%d" % s])
        P.op("dve", lambda e: e.tensor_scalar(out=xn[0:rows, :], in0=xt[0:rows, :], scalar1=st[0:rows, 2:3], scalar2=None, op0=ALU.mult),
             reads=["xt%d" % s, "st%d" % s], writes=["xn%d" % s])
        pb = d.ps[6 + s][:, :].bitcast(BF16)
        for kc in range(8):
            P.op("pe", lambda e, kc=kc: e.transpose(out=pb[:, kc * 128:kc * 128 + rows], in_=xn[0:rows, kc * 128:(kc + 1) * 128],
                                                    identity=d.ident_b[0:rows, 0:rows]),
                 reads=["xn%d" % s, "ident_b"], writes=["ps%d" % (6 + s)])
        for kc in range(8):
            P.op("dve", lambda e, kc=kc: e.tensor_scalar(out=hT_dst[:, kc, :], in0=pb[:, kc * 128:kc * 128 + rows],
                                                         scalar1=A[:, kc:kc + 1], scalar2=sh[:, kc:kc + 1], op0=ALU.mult, op1=ALU.add),
                 reads=["ps%d" % (6 + s), "A", "cols"], writes=["hT"])

    def stage_a(self, l):
        d, P = self, self.P
        x_src = d.x_in if l == 0 else d.XRES
        cols = d.mod_cols(l)
        A1, sh1 = d.norm_setup(l, cols, 0, d.n1g)
        fb = d.carve([NH], F32)
        P.dma("sp", fb, d.fbias[l:l + 1, :].partition_broadcast(128), writes=["fb"])
        W = d.carve([8, NW], BF16)
        wsrc = d.w_in[l].rearrange("(kc p) n -> p kc n", p=128)
        for (s0, d0, n) in ((0, 0, 1224), (SB_QF, O_QF, 1536 + 8), (SB_G, O_G, 2048)):
            for c0 in range(0, n, 512):
                c1 = min(n, c0 + 512)
                P.dma("pool", W[:, :, s0 + c0:s0 + c1], wsrc[:, :, d0 + c0:d0 + c1], reads=["w_in"], writes=["W"])
        nb = d.norm_bufs()
        hT = d.carve([8, 512], BF16)
        fm = [d.carve([512], BF16) for _ in range(4)]
        qa = [d.carve([512], BF16) for _ in range(2)]
        qf32 = [d.carve([512], F32) for _ in range(2)]
        rt = d.carve([6, 64], F32)
        tT = [d.carve([4, 128], BF16) for _ in range(2)]
        vf = [d.carve([512], BF16) for _ in range(2)]
        kk = d.carve([256], BF16)
        kT = d.carve([2, 128], BF16)
        sm = d.carve([208], F32)
        va = d.carve([64], BF16)
        wi = d.carve([NH], F32)
        lf = d.carve([NH], F32)
        cl = d.carve([NH], F32)
        PS = d.ps

        parts = self.dbg.get('a_parts', ('fm', 'q', 'vf', 'small'))
        for g in range(self.dbg.get('a_groups', NT // 4)):
            for tt in range(4):
                t = 4 * g + tt
                d.norm_tile(nb, t % 2, 128, x_src[t * 128:(t + 1) * 128, :], "xres%d" % t, A1, sh1, hT[:, :, tt * 128:(tt + 1) * 128])
            fmjobs = [(SB_QF + 128 * i, d.QFT, i, None) for i in range(4)] + \
                     [(SB_KF + 128 * i, d.KFT, i, None) for i in range(4)] + \
                     [(SB_G + 128 * i, d.GT, i, AF.Sigmoid) for i in range(16)]
            for ji, (c0, dst, bi, fn) in enumerate(fmjobs if 'fm' in parts else []):
                b = ji % 4
                for kc in range(8):
                    P.op("pe", lambda e, kc=kc, c0=c0, b=b: e.matmul(PS[b][:, :], lhsT=W[:, kc, c0:c0 + 128], rhs=hT[:, kc, :],
                                                                   start=(kc == 0), stop=(kc == 7)),
                         reads=["W", "hT"], writes=["ps%d" % b])
                P.op("act", lambda e, b=b, fn=fn: e.activation(out=fm[b], in_=PS[b][:, :], func=(fn or AF.Copy)),
                     reads=["ps%d" % b], writes=["fm%d" % b])
                P.dma("pool", dst[bi, :, g * 512:(g + 1) * 512], fm[b], reads=["fm%d" % b], writes=[self.key("dr")])
            for tt in range(4):
                t = 4 * g + tt
                tok = slice(t * 128, (t + 1) * 128)
                hTt = hT[:, :, tt * 128:(tt + 1) * 128]

                def proj(bank, c0, n, hTt=hTt):
                    for kc in range(8):
                        P.op("pe", lambda e, kc=kc: e.matmul(PS[bank][:, 0:n], lhsT=hTt[:, kc, :], rhs=W[:, kc, c0:c0 + n],
                                                             start=(kc == 0), stop=(kc == 7)),
                             reads=["W", "hT"], writes=["ps%d" % bank])

                cs_t = d.cs[:, t, :]
                sn_t = d.sn[:, t, :]

                def rope(src, dst, nh, skey, dkey):
                    s3 = src.rearrange("p (h x) -> p h x", x=64)
                    d3 = dst.rearrange("p (h x) -> p h x", x=64)
                    x1, x2 = s3[:, :, 0:8], s3[:, :, 8:16]
                    cb = cs_t.unsqueeze(1).to_broadcast([128, nh, 8])
                    sb_ = sn_t.unsqueeze(1).to_broadcast([128, nh, 8])
                    r = [rt[:, i, 0:nh * 8].rearrange("p (h x) -> p h x", x=8) for i in range(4)]
                    P.op("dve", lambda e: e.tensor_tensor(out=r[0], in0=x1, in1=cb, op=ALU.mult), reads=[skey, "cs"], writes=["rt0"])
                    P.op("dve", lambda e: e.tensor_tensor(out=r[1], in0=x2, in1=sb_, op=ALU.mult), reads=[skey, "sn"], writes=["rt1"])
                    P.op("dve", lambda e: e.tensor_tensor(out=r[2], in0=x2, in1=cb, op=ALU.mult), reads=[skey, "cs"], writes=["rt2"])
                    P.op("dve", lambda e: e.tensor_tensor(out=r[3], in0=x1, in1=sb_, op=ALU.mult), reads=[skey, "sn"], writes=["rt3"])
                    P.op("dve", lambda e: e.tensor_tensor(out=d3[:, :, 0:8], in0=r[0], in1=r[1], op=ALU.subtract),
                         reads=["rt0", "rt1"], writes=[dkey])
                    P.op("dve", lambda e: e.tensor_tensor(out=d3[:, :, 8:16], in0=r[2], in1=r[3], op=ALU.add),
                         reads=["rt2", "rt3"], writes=[dkey])

                for qi_, (c0, dst) in enumerate(((SB_QA, d.QAT), (SB_QI, d.QIT)) if 'q' in parts else ()):
                    bank = 4 + qi_
                    proj(bank, c0, 512)
                    qs = qa[qi_]
                    qf = qf32[qi_]
                    P.op("act", lambda e, bank=bank, qf=qf: e.activation(out=qf, in_=PS[bank][:, :], func=AF.Copy),
                         reads=["ps%d" % bank], writes=["qf%d" % qi_])
                    P.op("pool", lambda e, qs=qs, qf=qf: e.tensor_copy(out=qs, in_=qf), reads=["qf%d" % qi_], writes=["qa%d" % qi_])
                    qsub = self.dbg.get("q_sub", 4)
                    if qsub >= 2:
                        rope(qf, qs, 8, "qf%d" % qi_, "qa%d" % qi_)
                    pb = PS[6 + qi_][:, :].bitcast(BF16)
                    if qsub >= 3:
                        for blk in range(4):
                            P.op("pe", lambda e, blk=blk, qs=qs, pb=pb: e.transpose(out=pb[:, blk * 128:(blk + 1) * 128],
                                                                                 in_=qs[:, blk * 128:(blk + 1) * 128], identity=d.ident_b[:]),
                                 reads=["qa%d" % qi_, "ident_b"], writes=["ps%d" % (6 + qi_)])
                        P.op("act", lambda e, pb=pb, qi_=qi_: e.activation(out=tT[qi_], in_=pb[:, 0:512].rearrange("p (a b) -> p a b", b=128),
                                                                           func=AF.Copy),
                             reads=["ps%d" % (6 + qi_)], writes=["tT%d" % qi_])
                    if qsub >= 4:
                        P.dma("pool", dst[:, :, tok].rearrange("a p t -> p a t"), tT[qi_], reads=["tT%d" % qi_], writes=[self.key("dr")])
                s = t % 2
                if 'vf' in parts:
                    proj(s, SB_VF, 512)
                    P.op("act", lambda e, s=s: e.activation(out=vf[s], in_=PS[s][:, :], func=AF.Copy), reads=["ps%d" % s], writes=["vf%d" % s])
                    P.dma("pool", d.VF[tok, :], vf[s], reads=["vf%d" % s], writes=[self.key("dr")])
                if 'small' not in parts:
                    continue
                for (o0, o1, c0) in ((0, 128, SB_KA), (128, 200, SB_KI), (200, 208, SB_FF)):
                    for kc in range(8):
                        P.op("pe", lambda e, kc=kc, hTt=hTt, o0=o0, o1=o1, c0=c0: e.matmul(
                            PS[2][:, o0:o1], lhsT=hTt[:, kc, :], rhs=W[:, kc, c0:c0 + (o1 - o0)], start=(kc == 0), stop=(kc == 7)),
                            reads=["W", "hT"], writes=["ps2"])
                P.op("act", lambda e: e.activation(out=sm, in_=PS[2][:, 0:208], func=AF.Copy), reads=["ps2"], writes=["sm"])
                P.op("dve", lambda e: e.tensor_copy(out=kk[:, 0:64], in_=sm[:, 0:64]), reads=["sm"], writes=["kk"])
                rope(sm[:, 0:64], kk[:, 0:64], 1, "sm", "kk")
                P.op("dve", lambda e: e.tensor_copy(out=kk[:, 64:128], in_=kk[:, 0:64]), reads=["kk"], writes=["kk"])
                P.op("dve", lambda e: e.tensor_copy(out=kk[:, 128:192], in_=sm[:, 128:192]), reads=["sm"], writes=["kk"])
                rope(sm[:, 128:192], kk[:, 128:192], 1, "sm", "kk")
                P.op("dve", lambda e: e.tensor_copy(out=kk[:, 192:256], in_=kk[:, 128:192]), reads=["kk"], writes=["kk"])
                pb = PS[3][:, :].bitcast(BF16)
                for i in range(2):
                    P.op("pe", lambda e, i=i, pb=pb: e.transpose(out=pb[:, i * 128:(i + 1) * 128], in_=kk[:, i * 128:(i + 1) * 128],
                                                                 identity=d.ident_b[:]), reads=["kk", "ident_b"], writes=["ps3"])
                P.op("act", lambda e, pb=pb: e.activation(out=kT, in_=pb[:, 0:256].rearrange("p (a b) -> p a b", b=128), func=AF.Copy),
                     reads=["ps3"], writes=["kT"])
                P.dma("pool", d.KAT2[:, tok], kT[:, 0, :], reads=["kT"], writes=[self.key("dr")])
                P.dma("pool", d.KIT2[:, tok], kT[:, 1, :], reads=["kT"], writes=[self.key("dr")])
                P.op("dve", lambda e: e.tensor_copy(out=va, in_=sm[:, 64:128]), reads=["sm"], writes=["va"])
                P.dma("pool", d.VA[tok, :], va, reads=["va"], writes=[self.key("dr")])
                P.op("dve", lambda e: e.tensor_scalar(out=wi, in0=sm[:, 192:200], scalar1=float(NH ** -0.5 * HD ** -0.5), scalar2=None,
                                                      op0=ALU.mult), reads=["sm"], writes=["wi"])
                P.dma("pool", d.WI[tok, :], wi, reads=["wi"], writes=[self.key("dr")])
                P.op("dve", lambda e: e.tensor_tensor(out=lf, in0=sm[:, 200:208], in1=fb, op=ALU.add), reads=["sm", "fb"], writes=["lf"])
                P.op("act", lambda e: e.activation(out=lf, in_=lf, func=AF.Exp, scale=-1.0), reads=["lf"], writes=["lf"])
                P.op("act", lambda e: e.activation(out=lf, in_=lf, func=AF.Ln, bias=1.0), reads=["lf"], writes=["lf"])
                P.op("dve", lambda e: e.tensor_scalar(out=lf, in0=lf, scalar1=-1.0, scalar2=None, op0=ALU.mult), reads=["lf"], writes=["lf"])
                P.op("pe", lambda e: e.matmul(PS[3][:, 256:264], lhsT=d.tri_f[:], rhs=lf, start=True, stop=True),
                     reads=["tri_f", "lf"], writes=["ps3"])
                P.op("dve", lambda e: e.tensor_copy(out=cl, in_=PS[3][:, 256:264]), reads=["ps3"], writes=["cl"])
                P.dma("pool", d.CUML[:, t * NH:(t + 1) * NH], cl, reads=["cl"], writes=[self.key("dr")])
        P.barrier()
        d.arena_reset()

    def gather(self, src2d, dst2d):
        rg = self.RG
        self.P.cc(lambda e: e.collective_compute("AllGather", ALU.bypass, replica_groups=rg, ins=[src2d], outs=[dst2d]),
                  writes=[self.key("cc")])

    def stage_gather(self, l):
        d = self
        for blk in range(4):
            for hf in range(2):
                d.gather(d.KFT[blk, hf * 64:(hf + 1) * 64, :], d.G_KFT_p[2 * blk + hf].rearrange("r p t -> (r p) t"))
        for i in range(NT // d.CHV):
            d.gather(d.VF[i * d.CHV * 128:(i + 1) * d.CHV * 128, :], d.G_VF_p[i].rearrange("r s c -> (r s) c"))
        for hf in range(2):
            d.gather(d.KAT2[hf * 64:(hf + 1) * 64, :], d.G_KAT2_p[hf].rearrange("r p t -> (r p) t"))
            d.gather(d.KIT2[hf * 64:(hf + 1) * 64, :], d.G_KIT2_p[hf].rearrange("r p t -> (r p) t"))
        d.gather(d.VA, d.G_VA.rearrange("r s c -> (r s) c"))
        d.gather(d.CUML, d.G_CUML.rearrange("r p t -> (r p) t"))
        d.P.barrier()

    def stage_halo(self, l):
        d = self
        d.gather(d.HALO, d.G_HALO.rearrange("r s c -> (r s) c"))
        d.P.barrier()

    def stage_b(self, l):
        d, P = self, self.P
        PS = d.ps
        CH = d.CHV
        CUMG = d.carve([2, NT, NH], F32)
        OFFB = d.carve([2, NT, NH], F32)
        tot = d.carve([NH], F32)
        offs = d.carve([NH], F32)
        su2 = d.carve([NGT], F32)
        K_sb = d.carve([4, 2, SL], BF16)
        V_sb = d.carve([2 * NT, 512], BF16)
        QF = [d.carve([4, 128], BF16) for _ in range(2)]
        bias = [d.carve([2, NT], F32) for _ in range(2)]
        E = [d.carve([128], BF16) for _ in range(3)]
        rec = d.carve([128], F32)
        oB = [d.carve([NH, 128], BF16) for _ in range(2)]
        P.dma("sp", su2[0:NGT, :], d.su2_in[:, :], writes=["su2"])
        for r in range(2):
            P.dma("sp", CUMG[:, r, :, :], d.G_CUML[r].rearrange("p (i h) -> p i h", h=NH), writes=["CUMG"])
            P.dma("sp", tot[r * NT:(r + 1) * NT, :], d.G_CUML[r, 127:128, :].rearrange("o (i h) -> (o i) h", h=NH), writes=["tot"])
            for blk in range(4):
                for hf in range(2):
                    P.dma("sp", K_sb[hf * 64:(hf + 1) * 64, blk, r, :], d.G_KFT_p[2 * blk + hf][r], writes=["K_sb"])
            for c0 in range(0, NT, CH):
                P.dma("sp", V_sb[:, r * NT + c0:r * NT + c0 + CH, :],
                      d.G_VF_p[c0 // CH][r].rearrange("(i p) c -> p i c", p=128), writes=["V_sb"])
        P.op("pe", lambda e: e.matmul(PS[0][0:NGT, 0:NH], lhsT=su2[0:NGT, :], rhs=tot[0:NGT, :], start=True, stop=True),
             reads=["su2", "tot"], writes=["ps0"])
        P.op("dve", lambda e: e.tensor_copy(out=offs[0:NGT, :], in_=PS[0][0:NGT, 0:NH]), reads=["ps0"], writes=["offs"])
        P.dma("sp", d.OFF[:, :], offs[0:NGT, :], reads=["offs"], writes=["OFF"])
        P.dma("sp", OFFB.rearrange("p r i h -> p (r i h)"),
              d.OFF.rearrange("g h -> (g h)").rearrange("(o n) -> o n", o=1).partition_broadcast(128), reads=["OFF"], writes=["OFFB"])
        P.op("dve", lambda e: e.tensor_tensor(out=CUMG.rearrange("p r i h -> p (r i h)"), in0=CUMG.rearrange("p r i h -> p (r i h)"),
                                              in1=OFFB.rearrange("p r i h -> p (r i h)"), op=ALU.add),
             reads=["CUMG", "OFFB"], writes=["CUMG"])
        LOOK = 2
        for i in range(NT):
            tok = slice(i * 128, (i + 1) * 128)
            qs = i % 2
            P.dma("sp", QF[qs], d.QFT[:, :, tok].rearrange("a p t -> p a t"), writes=["QF%d" % qs])
            tiles = [(r, ii) for ii in range(i + 1) for r in range(2)]
            n = len(tiles)
            for h in range(NH):
                hh, blk = h % 2, h // 2
                pp = slice(hh * 64, hh * 64 + 64)
                bs = h % 2
                bt = bias[bs]
                P.op("dve", lambda e, bt=bt, h=h, i=i: e.tensor_scalar(out=bt[:, :, 0:i + 1], in0=CUMG[:, :, 0:i + 1, h], scalar1=-1.0,
                                                                      scalar2=OFFB[:, 0, i, h:h + 1], op0=ALU.mult, op1=ALU.add),
                     reads=["CUMG", "OFFB"], writes=["bias%d" % bs])
                po, pd = PS[4 + bs], PS[6 + bs]

                def qk(j, pp=pp, blk=blk, qs=qs):
                    r, ii = tiles[j]
                    b = j % 4
                    P.op("pe", lambda e: e.matmul(PS[b][:, 0:128], lhsT=K_sb[pp, blk, r, ii * 128:(ii + 1) * 128], rhs=QF[qs][pp, blk, :],
                                                  start=True, stop=True), reads=["K_sb", "QF%d" % qs], writes=["ps%d" % b])

                for j in range(min(LOOK, n)):
                    qk(j)
                for j in range(n):
                    r, ii = tiles[j]
                    b, es = j % 4, j % 3
                    P.op("act", lambda e, b=b, es=es, bt=bt, r=r, ii=ii: e.activation(out=E[es], in_=PS[b][:, 0:128], func=AF.Exp,
                                                                                     scale=float(HD ** -0.5), bias=bt[:, r, ii:ii + 1]),
                         reads=["ps%d" % b, "bias%d" % bs], writes=["E%d" % es])
                    if j + LOOK < n:
                        qk(j + LOOK)
                    if ii == i:
                        P.op("dve", lambda e, es=es, r=r: e.tensor_tensor(out=E[es], in0=E[es], in1=d.fmask[:, r, :], op=ALU.mult),
                             reads=["E%d" % es, "fmask"], writes=["E%d" % es])
                    P.op("pe", lambda e, es=es, r=r, ii=ii, h=h, j=j, po=po: e.matmul(
                        po[0:64, 0:128], lhsT=V_sb[:, r * NT + ii, h * 64:(h + 1) * 64], rhs=E[es], start=(j == 0), stop=(j == n - 1)),
                        reads=["V_sb", "E%d" % es], writes=["ps%d" % (4 + bs)])
                    P.op("pe", lambda e, es=es, j=j, pd=pd: e.matmul(pd[0:64, 0:128], lhsT=d.ones_b[:, :], rhs=E[es],
                                                                    start=(j == 0), stop=(j == n - 1)),
                         reads=["ones_b", "E%d" % es], writes=["ps%d" % (6 + bs)])
                P.op("dve", lambda e, pd=pd: e.reciprocal(out=rec[0:64, :], in_=pd[0:64, 0:128]), reads=["ps%d" % (6 + bs)], writes=["rec"])
                P.op("dve", lambda e, po=po, h=h, qs=qs: e.tensor_tensor(out=oB[qs][0:64, h, :], in0=po[0:64, 0:128], in1=rec[0:64, :],
                                                                        op=ALU.mult),
                     reads=["ps%d" % (4 + bs), "rec"], writes=["oB%d" % qs])
            P.dma("pool", d.OBT[:, :, tok], oB[qs][0:64, :, :], reads=["oB%d" % qs], writes=[self.key("dr")])
        P.barrier()
        d.arena_reset()

    def stage_s(self, l):
        d, P = self, self.P
        PS = d.ps
        CH = min(8, NT)
        x_src = d.x_in if l == 0 else d.XRES
        KI = d.carve([2, SL], BF16)
        KA = d.carve([2, SL], BF16)
        VA = d.carve([2 * NT, HD], BF16)
        WIs = d.carve([NT, NH], F32)
        Wa = d.carve([NH, D], BF16)
        Wb = d.carve([NH, D], BF16)
        Wo = d.carve([8, D], BF16)
        G1B = d.carve([D], F32)
        SC = d.carve([2, SL], F32)
        MK = d.carve([2, SL], BF16)
        QI = d.carve([4, 128], BF16)
        QA = d.carve([4, 128], BF16)
        Dg = d.carve([NH, 128], BF16)
        R = [d.carve([512], BF16) for _ in range(3)]
        bv = d.carve([16], F32)
        mT = [d.carve([128], BF16) for _ in range(2)]
        Eb = [d.carve([4, 128], BF16) for _ in range(2)]
        Pm = [d.carve([4, 128], BF16) for _ in range(2)]
        rec = d.carve([512], F32)
        oA = d.carve([NH, 128], BF16)
        oBs = d.carve([NH, 128], BF16)
        Gt = d.carve([16, 128], BF16)
        t1 = d.carve([128], F32)
        t2 = d.carve([128], F32)
        mg = d.carve([8, 128], BF16)
        xt = d.carve([D], F32)
        xo = d.carve([D], F32)
        for r in range(2):
            for hf in range(2):
                P.dma("sp", KI[hf * 64:(hf + 1) * 64, r, :], d.G_KIT2_p[hf][r], writes=["KI"])
                P.dma("sp", KA[hf * 64:(hf + 1) * 64, r, :], d.G_KAT2_p[hf][r], writes=["KA"])
            for c0 in range(0, NT, CH):
                P.dma("sp", VA[:, r * NT + c0:r * NT + c0 + CH, :],
                      d.G_VA[r, c0 * 128:(c0 + CH) * 128, :].rearrange("(i p) c -> p i c", p=128), writes=["VA"])
        for c0 in range(0, NT, CH):
            P.dma("sp", WIs[:, c0:c0 + CH, :], d.WI[c0 * 128:(c0 + CH) * 128, :].rearrange("(i p) h -> p i h", p=128), writes=["WIs"])
        P.dma("pool", Wa[0:64, :, :], d.w_a[l].rearrange("(h dd) n -> dd h n", dd=64), reads=["w_a"], writes=["Wa"])
        P.dma("pool", Wb[0:64, :, :], d.w_b[l].rearrange("(h dd) n -> dd h n", dd=64), reads=["w_b"], writes=["Wb"])
        P.dma("pool", Wo, d.w_o[l].rearrange("(kc p) n -> p kc n", p=128), reads=["w_o"], writes=["Wo"])
        P.dma("sp", G1B, d.modv[l:l + 1, 2 * D:3 * D].partition_broadcast(128), writes=["G1B"])
        lo, hi, mid, cnt, cc_, d1, d2, rmx, rmn = [bv[:, k:k + 1] for k in range(9)]
        scale = float(HD ** -0.5)

        for i in range(NT):
            tok = slice(i * 128, (i + 1) * 128)
            ncol = (i + 1) * 128
            P.dma("sp", QI, d.QIT[:, :, tok].rearrange("a p t -> p a t"), writes=["QI"])
            P.dma("sp", QA, d.QAT[:, :, tok].rearrange("a p t -> p a t"), writes=["QA"])
            P.dma("sp", oBs[0:64, :, :], d.OBT[:, :, tok], writes=["oBs"])
            P.dma("sp", Gt, d.GT[:, :, tok].rearrange("a p t -> p a t"), writes=["Gt"])
            P.dma("sp", xt, x_src[tok, :], reads=["xres%d" % i], writes=["xt"])
            for h in range(NH):
                P.op("dve", lambda e, h=h, i=i: e.tensor_scalar(out=Dg[:, h, :], in0=d.ident_b[:], scalar1=WIs[:, i, h:h + 1], scalar2=None,
                                                                op0=ALU.mult), reads=["ident_b", "WIs"], writes=["Dg"])
            for r in range(2):
                for c0 in range(0, ncol, 512):
                    c1 = min(ncol, c0 + 512)
                    w = c1 - c0

                    def iqk(h, r=r, c0=c0, c1=c1, w=w):
                        hh, blk = h % 2, h // 2
                        pp = slice(hh * 64, hh * 64 + 64)
                        b = 4 + h % 3
                        P.op("pe", lambda e: e.matmul(PS[b][:, 0:w], lhsT=QI[pp, blk, :], rhs=KI[pp, r, c0:c1], start=True, stop=True),
                             reads=["QI", "KI"], writes=["ps%d" % b])

                    iqk(0)
                    iqk(1)
                    for h in range(NH):
                        b, rs = 4 + h % 3, h % 3
                        P.op("act", lambda e, b=b, rs=rs, w=w: e.activation(out=R[rs][:, 0:w], in_=PS[b][:, 0:w], func=AF.Relu),
                             reads=["ps%d" % b], writes=["R%d" % rs])
                        if h + 2 < NH:
                            iqk(h + 2)
                        P.op("pe", lambda e, h=h, rs=rs, w=w: e.matmul(PS[7][:, 0:w], lhsT=Dg[:, h, :], rhs=R[rs][:, 0:w],
                                                                      start=(h == 0), stop=(h == NH - 1)),
                             reads=["Dg", "R%d" % rs], writes=["ps7"])
                    P.op("dve", lambda e, r=r, c0=c0, c1=c1, w=w: e.tensor_copy(out=SC[:, r, c0:c1], in_=PS[7][:, 0:w]),
                         reads=["ps7"], writes=["SC"])
            SCv = SC[:, :, 0:ncol]
            MKv = MK[:, :, 0:ncol]
            P.op("dve", lambda e, SCv=SCv, MKv=MKv: e.tensor_scalar(out=MKv, in0=SCv, scalar1=1.0, scalar2=None, op0=ALU.mult, op1=ALU.max,
                                                                    accum_out=rmx), reads=["SC"], writes=["MK", "rmx"])
            P.op("dve", lambda e, SCv=SCv, MKv=MKv: e.tensor_scalar(out=MKv, in0=SCv, scalar1=1.0, scalar2=None, op0=ALU.mult, op1=ALU.min,
                                                                    accum_out=rmn), reads=["SC"], writes=["MK", "rmn"])
            for r in range(2):
                P.op("dve", lambda e, r=r, i=i: e.tensor_tensor(out=SC[:, r, i * 128:(i + 1) * 128], in0=SC[:, r, i * 128:(i + 1) * 128],
                                                                in1=d.madd[:, r, :], op=ALU.add), reads=["SC", "madd"], writes=["SC"])
            P.op("dve", lambda e: e.tensor_scalar(out=lo, in0=rmn, scalar1=-1.0, scalar2=None, op0=ALU.add), reads=["rmn"], writes=["lo"])
            P.op("dve", lambda e: e.tensor_scalar(out=hi, in0=rmx, scalar1=1.0, scalar2=None, op0=ALU.add), reads=["rmx"], writes=["hi"])
            for it in range(self.dbg.get("n_bisect", N_BISECT)):
                P.op("dve", lambda e: e.tensor_tensor(out=mid, in0=lo, in1=hi, op=ALU.add), reads=["lo", "hi"], writes=["mid"])
                P.op("dve", lambda e: e.tensor_scalar(out=mid, in0=mid, scalar1=0.5, scalar2=None, op0=ALU.mult), reads=["mid"], writes=["mid"])
                P.op("dve", lambda e, SCv=SCv, MKv=MKv: e.tensor_scalar(out=MKv, in0=SCv, scalar1=mid, scalar2=None, op0=ALU.is_gt,
                                                                        op1=ALU.add, accum_out=cnt), reads=["SC", "mid"], writes=["MK", "cnt"])
                P.op("dve", lambda e: e.tensor_single_scalar(out=cc_, in_=cnt, scalar=TOPK - 0.5, op=ALU.is_ge), reads=["cnt"], writes=["cc"])
                P.op("dve", lambda e: e.tensor_tensor(out=d1, in0=mid, in1=lo, op=ALU.subtract), reads=["mid", "lo"], writes=["d1"])
                P.op("dve", lambda e: e.tensor_tensor(out=d2, in0=hi, in1=mid, op=ALU.subtract), reads=["mid", "hi"], writes=["d2"])
                P.op("dve", lambda e: e.scalar_tensor_tensor(out=lo, in0=d1, scalar=cc_, in1=lo, op0=ALU.mult, op1=ALU.add),
                     reads=["d1", "cc", "lo"], writes=["lo"])
                P.op("dve", lambda e: e.scalar_tensor_tensor(out=hi, in0=d2, scalar=cc_, in1=mid, op0=ALU.mult, op1=ALU.add),
                     reads=["d2", "cc", "mid"], writes=["hi"])
            P.op("dve", lambda e, SCv=SCv, MKv=MKv: e.tensor_scalar(out=MKv, in0=SCv, scalar1=lo, scalar2=None, op0=ALU.is_gt),
                 reads=["SC", "lo"], writes=["MK"])
            tiles = [(r, ii) for ii in range(i + 1) for r in range(2)]
            n = len(tiles)
            for j, (r, ii) in enumerate(tiles):
                ms = j % 2
                pbm = PS[6][:, :].bitcast(BF16)
                P.op("pe", lambda e, r=r, ii=ii, ms=ms, pbm=pbm: e.transpose(out=pbm[:, ms * 128:(ms + 1) * 128],
                                                                          in_=MK[:, r, ii * 128:(ii + 1) * 128], identity=d.ident_b[:]),
                     reads=["MK", "ident_b"], writes=["ps6"])
                P.op("act", lambda e, ms=ms, pbm=pbm: e.activation(out=mT[ms], in_=pbm[:, ms * 128:(ms + 1) * 128], func=AF.Copy),
                     reads=["ps6"], writes=["mT%d" % ms])
                for hh in range(2):
                    pp = slice(hh * 64, hh * 64 + 64)
                    P.op("pe", lambda e, pp=pp, r=r, ii=ii, hh=hh: e.matmul(PS[4 + hh][:, :], lhsT=KA[pp, r, ii * 128:(ii + 1) * 128],
                                                                            rhs=QA[pp, :, :], start=True, stop=True),
                         reads=["KA", "QA"], writes=["ps%d" % (4 + hh)])
                    P.op("act", lambda e, hh=hh: e.activation(out=Eb[hh], in_=PS[4 + hh][:, :].rearrange("p (a b) -> p a b", b=128),
                                                              func=AF.Exp, scale=scale), reads=["ps%d" % (4 + hh)], writes=["Eb%d" % hh])
                    P.op("pool", lambda e, hh=hh, ms=ms: e.tensor_tensor(out=Pm[hh], in0=Eb[hh],
                                                                         in1=mT[ms].unsqueeze(1).to_broadcast([128, 4, 128]), op=ALU.mult),
                         reads=["Eb%d" % hh, "mT%d" % ms], writes=["Pm%d" % hh])
                    P.op("pe", lambda e, hh=hh, r=r, ii=ii, j=j: e.matmul(PS[hh][0:64, :], lhsT=VA[:, r * NT + ii, :], rhs=Pm[hh],
                                                                          start=(j == 0), stop=(j == n - 1)),
                         reads=["VA", "Pm%d" % hh], writes=["ps%d" % hh])
                    P.op("pe", lambda e, hh=hh, j=j: e.matmul(PS[2 + hh][0:64, :], lhsT=d.ones_b[:, :], rhs=Pm[hh],
                                                              start=(j == 0), stop=(j == n - 1)),
                         reads=["ones_b", "Pm%d" % hh], writes=["ps%d" % (2 + hh)])
            for hh in range(2):
                P.op("dve", lambda e, hh=hh: e.reciprocal(out=rec[0:64, :], in_=PS[2 + hh][0:64, :]), reads=["ps%d" % (2 + hh)], writes=["rec"])
                P.op("dve", lambda e, hh=hh: e.tensor_tensor(out=oA[0:64, hh::2, :] if False else oA[0:64, :, :].rearrange(
                    "p (b two) t -> p b two t", two=2)[:, :, hh, :], in0=PS[hh][0:64, :].rearrange("p (b t) -> p b t", t=128),
                    in1=rec[0:64, :].rearrange("p (b t) -> p b t", t=128), op=ALU.mult),
                    reads=["ps%d" % hh, "rec"], writes=["oA"])
            for fb in range(8):
                fs = slice(fb * 128, (fb + 1) * 128)
                for h in range(NH):
                    P.op("pe", lambda e, h=h, fs=fs: e.matmul(PS[7][:, 0:128], lhsT=Wa[0:64, h, fs], rhs=oA[0:64, h, :],
                                                              start=(h == 0), stop=(h == NH - 1)), reads=["Wa", "oA"], writes=["ps7"])
                for h in range(NH):
                    P.op("pe", lambda e, h=h, fs=fs: e.matmul(PS[7][:, 128:256], lhsT=Wb[0:64, h, fs], rhs=oBs[0:64, h, :],
                                                              start=(h == 0), stop=(h == NH - 1)), reads=["Wb", "oBs"], writes=["ps7"])
                P.op("dve", lambda e, fb=fb: e.tensor_tensor(out=t1, in0=PS[7][:, 0:128], in1=Gt[:, fb, :], op=ALU.mult),
                     reads=["ps7", "Gt"], writes=["t1"])
                P.op("dve", lambda e, fb=fb: e.tensor_tensor(out=t2, in0=PS[7][:, 128:256], in1=Gt[:, 8 + fb, :], op=ALU.mult),
                     reads=["ps7", "Gt"], writes=["t2"])
                P.op("dve", lambda e, fb=fb: e.tensor_tensor(out=mg[:, fb, :], in0=t1, in1=t2, op=ALU.add),
                     reads=["t1", "t2"], writes=["mg"])
            for half in range(2):
                hs = slice(half * 512, (half + 1) * 512)
                for kc in range(8):
                    P.op("pe", lambda e, kc=kc, hs=hs, half=half: e.matmul(PS[4 + half][:, :], lhsT=mg[:, kc, :], rhs=Wo[:, kc, hs],
                                                                           start=(kc == 0), stop=(kc == 7)),
                         reads=["mg", "Wo"], writes=["ps%d" % (4 + half)])
                P.op("dve", lambda e, hs=hs, half=half: e.tensor_tensor(out=xo[:, hs], in0=PS[4 + half][:, :], in1=G1B[:, hs], op=ALU.mult),
                     reads=["ps%d" % (4 + half), "G1B"], writes=["xo"])
                P.op("dve", lambda e, hs=hs: e.tensor_tensor(out=xo[:, hs], in0=xo[:, hs], in1=xt[:, hs], op=ALU.add),
                     reads=["xo", "xt"], writes=["xo"])
            P.dma("pool", d.XRES[tok, :], xo, reads=["xo"], writes=["xres%d" % i])
            P.dma("pool", d.HALO[2 * i:2 * i + 2, :], xo[126:128, :], reads=["xo"], writes=[self.key("dr")])
        P.barrier()
        d.arena_reset()

    def stage_d(self, l):
        d, P = self, self.P
        PS = d.ps
        last = (l == DEPTH - 1)
        NC2 = 2 * NFF
        cols = d.mod_cols(l)
        A2, sh2 = d.norm_setup(l, cols, 3, d.n2g)
        Wu = d.carve([8, 2 * DFF], BF16)
        Wd = d.carve([NFF, D], BF16)
        wsrc = d.w_up[l].rearrange("(kc p) n -> p kc n", p=128)
        for c0 in range(0, 2 * DFF, 512):
            P.dma("pool", Wu[:, :, c0:c0 + 512], wsrc[:, :, c0:c0 + 512], reads=["w_up"], writes=["Wu"])
        dsrc = d.w_dn[l].rearrange("(j p) n -> p j n", p=128)
        for j0 in range(0, NFF, 2):
            P.dma("pool", Wd[:, j0:j0 + 2, :], dsrc[:, j0:j0 + 2, :], reads=["w_dn"], writes=["Wd"])
        cw = d.carve([3, NC2], F32)
        cb = d.carve([NC2], F32)
        G2B = d.carve([D], F32)
        FGB = d.carve([D], F32)
        hm = d.carve([2 * NT], F32)
        P.dma("sp", cw, d.convw[l], writes=["cw"])
        P.dma("sp", cb, d.convb[l], writes=["cb"])
        P.dma("sp", G2B, d.modv[l:l + 1, 5 * D:6 * D].partition_broadcast(128), writes=["G2B"])
        P.dma("sp", FGB, d.fgain[0:1, :].partition_broadcast(128), writes=["FGB"])
        P.dma("sp", hm, d.hmask_in[:, :], writes=["hm"])
        nb = d.norm_bufs()
        GW = 2
        GC = GW * 128
        hT = d.carve([8, max(GC, 2 * NT)], BF16)
        uh = d.carve([NC2, 2 * NT], F32)
        ub = [d.carve([GW, 130], F32) for _ in range(2)]
        vv = [d.carve([GW, 128], F32) for _ in range(2)]
        sa = d.carve([GC], F32)
        gT = d.carve([NFF, GC], BF16)
        hB = nb["xt"][1]
        xr = d.carve([D], F32)
        xo = d.carve([D], F32)
        xo2 = xr
        fst = d.carve([4], F32)
        R2 = 2 * NT
        xh = nb["xt"][0]
        P.dma("sp", xh[0:R2, :], d.G_HALO[0], writes=["xt0"])
        P.dma("sp", hB[0:2, :], d.G_HALO[1, 0:2, :], writes=["xt1"])
        if R2 > 2:
            P.dma("sp", hB[2:R2, :], d.G_HALO[1, 0:R2 - 2, :], writes=["xt1"])
        P.op("dve", lambda e: e.tensor_scalar(out=xh[0:R2, :], in0=xh[0:R2, :], scalar1=d.selc[0:R2, 0:1], scalar2=None, op0=ALU.mult),
             reads=["xt0", "selc"], writes=["xt0"])
        P.op("dve", lambda e: e.scalar_tensor_tensor(out=xh[0:R2, :], in0=hB[0:R2, :], scalar=d.selc[0:R2, 1:2], in1=xh[0:R2, :],
                                                     op0=ALU.mult, op1=ALU.add), reads=["xt1", "selc", "xt0"], writes=["xt0"])
        d.norm_tile(nb, 0, R2, None, None, A2, sh2, hT[:, :, 0:R2], load=False)
        for c in range(NC2):
            b = c % 4
            for kc in range(8):
                P.op("pe", lambda e, kc=kc, c=c, b=b: e.matmul(PS[b][:, 0:R2], lhsT=Wu[:, kc, c * 128:(c + 1) * 128], rhs=hT[:, kc, 0:R2],
                                                              start=(kc == 0), stop=(kc == 7)), reads=["Wu", "hT"], writes=["ps%d" % b])
            P.op("dve", lambda e, c=c, b=b: e.tensor_tensor(out=uh[:, c, :], in0=PS[b][:, 0:R2], in1=hm, op=ALU.mult),
                 reads=["ps%d" % b, "hm"], writes=["uh"])
        for g in range(NT // GW):
            for tt in range(GW):
                t = GW * g + tt
                d.norm_tile(nb, t % 2, 128, d.XRES[t * 128:(t + 1) * 128, :], "xres%d" % t, A2, sh2, hT[:, :, tt * 128:(tt + 1) * 128])
            for j in range(NFF):
                for w_, c in enumerate((j, NFF + j)):
                    b = (2 * j + w_) % 4
                    u = ub[w_]
                    v = vv[w_]
                    eng = "dve"
                    for kc in range(8):
                        P.op("pe", lambda e, kc=kc, c=c, b=b: e.matmul(PS[b][:, 0:GC], lhsT=Wu[:, kc, c * 128:(c + 1) * 128], rhs=hT[:, kc, 0:GC],
                                                                      start=(kc == 0), stop=(kc == 7)), reads=["Wu", "hT"], writes=["ps%d" % b])
                    P.op("act", lambda e, b=b, u=u: e.activation(out=u[:, :, 2:130], in_=PS[b][:, 0:GC].rearrange("p (a t) -> p a t", t=128),
                                                                 func=AF.Copy), reads=["ps%d" % b], writes=["ub%d" % w_])
                    P.op(eng, lambda e, u=u, c=c, g=g: e.tensor_copy(out=u[:, :, 0:2], in_=uh[:, c, 2 * GW * g:2 * GW * (g + 1)].rearrange(
                        "p (a t) -> p a t", t=2)), reads=["uh"], writes=["ub%d" % w_])
                    P.op(eng, lambda e, u=u, v=v, c=c: e.tensor_scalar(out=v, in0=u[:, :, 2:130], scalar1=cw[:, 2, c:c + 1],
                                                                       scalar2=cb[:, c:c + 1], op0=ALU.mult, op1=ALU.add),
                         reads=["ub%d" % w_, "cw", "cb"], writes=["vv%d" % w_])
                    P.op(eng, lambda e, u=u, v=v, c=c: e.scalar_tensor_tensor(out=v, in0=u[:, :, 1:129], scalar=cw[:, 1, c:c + 1], in1=v,
                                                                              op0=ALU.mult, op1=ALU.add),
                         reads=["ub%d" % w_, "cw", "vv%d" % w_], writes=["vv%d" % w_])
                    P.op(eng, lambda e, u=u, v=v, c=c: e.scalar_tensor_tensor(out=v, in0=u[:, :, 0:128], scalar=cw[:, 0, c:c + 1], in1=v,
                                                                              op0=ALU.mult, op1=ALU.add),
                         reads=["ub%d" % w_, "cw", "vv%d" % w_], writes=["vv%d" % w_])
                P.op("act", lambda e: e.activation(out=sa, in_=vv[0].rearrange("p a t -> p (a t)"), func=AF.Silu),
                     reads=["vv0"], writes=["sa"])
                P.op("dve", lambda e, j=j: e.tensor_tensor(out=gT[:, j, :], in0=sa, in1=vv[1].rearrange("p a t -> p (a t)"), op=ALU.mult),
                     reads=["sa", "vv1"], writes=["gT"])
            for tt in range(GW):
                t = GW * g + tt
                tok = slice(t * 128, (t + 1) * 128)
                P.dma("sp", xr, d.XRES[tok, :], reads=["xres%d" % t], writes=["xr"])
                for half in range(2):
                    hs = slice(half * 512, (half + 1) * 512)
                    for j in range(NFF):
                        P.op("pe", lambda e, j=j, hs=hs, half=half, tt=tt: e.matmul(
                            PS[4 + half][:, :], lhsT=gT[:, j, tt * 128:(tt + 1) * 128], rhs=Wd[:, j, hs], start=(j == 0), stop=(j == NFF - 1)),
                            reads=["gT", "Wd"], writes=["ps%d" % (4 + half)])
                    P.op("dve", lambda e, hs=hs, half=half: e.tensor_tensor(out=xo[:, hs], in0=PS[4 + half][:, :], in1=G2B[:, hs], op=ALU.mult),
                         reads=["ps%d" % (4 + half), "G2B"], writes=["xo"])
                    P.op("dve", lambda e, hs=hs: e.tensor_tensor(out=xo[:, hs], in0=xo[:, hs], in1=xr[:, hs], op=ALU.add),
                         reads=["xo", "xr"], writes=["xo"])
                if not last:
                    P.dma("pool", d.XRES[tok, :], xo, reads=["xo"], writes=["xres%d" % t])
                else:
                    P.op("act", lambda e: e.activation(out=xo2, in_=xo, func=AF.Square, accum_out=fst[:, 0:1]),
                         reads=["xo"], writes=["xr", "fst"])
                    P.op("act", lambda e: e.activation(out=fst[:, 1:2], in_=fst[:, 0:1], func=AF.Ln, scale=1.0 / D, bias=EPS),
                         reads=["fst"], writes=["fst"])
                    P.op("act", lambda e: e.activation(out=fst[:, 2:3], in_=fst[:, 1:2], func=AF.Exp, scale=-0.5),
                         reads=["fst"], writes=["fst"])
                    P.op("dve", lambda e: e.scalar_tensor_tensor(out=xo2, in0=xo, scalar=fst[:, 2:3], in1=FGB, op0=ALU.mult, op1=ALU.mult),
                         reads=["xo", "fst", "FGB"], writes=["xr"])
                    P.dma("pool", d.out[tok, :], xo2, reads=["xr"], writes=[self.key("dr")])
        P.barrier()
        d.arena_reset()


def host_inputs(inputs):
    x = np.asarray(inputs["x"], np.float32)
    c = np.asarray(inputs["c"], np.float32)
    pos = np.asarray(inputs["positions"], np.int32)
    nb = x.shape[0]
    ncores = 2 * nb
    f32 = lambda a: np.ascontiguousarray(np.asarray(a, np.float32))
    invf = (ROPE_THETA ** (-np.arange(0, 16, 2, dtype=np.float32) / 16)).astype(np.float32)
    gidx = np.array([2 * i + r for r in range(2) for i in range(NT)])
    su2 = (gidx[:, None] < gidx[None, :]).astype(np.float32)
    tri = np.triu(np.ones((128, 128), np.float32))
    ones = np.ones((128, 128), np.float32)
    zeros = np.zeros((128, 128), np.float32)
    qq = np.arange(128)[:, None] // 64
    ss = np.arange(128)[None, :] // 64
    cdiag = np.where(ss <= qq, 0.0, NEG).astype(np.float32)
    negs = np.full((128, 128), NEG, np.float32)
    common = {
        "invf": f32(np.broadcast_to(invf[None, :], (128, 8))),
        "ident": np.eye(128, dtype=np.float32),
        "tri": tri,
        "su2": su2,
        "cT_all": f32(c.reshape(nb, 8, 128).transpose(2, 1, 0)),
        "n1g": f32(np.asarray(inputs["norm1_g"], np.float32).reshape(DEPTH, 8, 128).transpose(0, 2, 1)),
        "n2g": f32(np.asarray(inputs["norm2_g"], np.float32).reshape(DEPTH, 8, 128).transpose(0, 2, 1)),
        "fbias": f32(inputs["forget_bias"]),
        "convw": f32(np.asarray(inputs["conv_w"], np.float32).reshape(DEPTH, 3, 2 * NFF, 128).transpose(0, 3, 1, 2)),
        "convb": f32(np.asarray(inputs["conv_b"], np.float32).reshape(DEPTH, 2 * NFF, 128).transpose(0, 2, 1)),
        "fgain": f32(np.asarray(inputs["final_g"], np.float32).reshape(1, D)),
    }
    wfull = {"w_in": inputs["w_in"], "w_a": inputs["w_branch_a"], "w_b": inputs["w_branch_b"], "w_o": inputs["w_out"],
             "w_up": inputs["w_up"], "w_dn": inputs["w_down"]}
    wflat = {k: np.asarray(v, np.float32).reshape(-1, np.asarray(v).shape[-1]) for k, v in wfull.items()}
    mod_w = np.asarray(inputs["mod_w"], np.float32)
    mod_b = np.asarray(inputs["mod_b"], np.float32)
    CW = 6 * D // ncores
    maps = []
    for core in range(ncores):
        b, r = core // 2, core % 2
        m = dict(common)
        m["x"] = f32(x[b].reshape(NGT, 128, D)[r::2].reshape(SL, D))
        m["pos"] = np.ascontiguousarray(pos[b].reshape(NGT, 128)[r::2].T.astype(np.int32))
        for k, v in wflat.items():
            n = v.shape[0] // ncores
            m[k + "_sh"] = f32(v[core * n:(core + 1) * n])
        m["mod_w_sh"] = f32(mod_w[:, :, core * CW:(core + 1) * CW])
        m["mod_b_sh"] = f32(mod_b[:, core * CW:(core + 1) * CW])
        bs = np.zeros((1, nb), np.float32)
        bs[0, b] = 1.0
        m["bsel"] = bs
        hmask = np.ones((128, 2 * NT), np.float32)
        if r == 0:
            m["fmask"] = np.stack([tri, zeros])
            m["madd"] = np.stack([cdiag, negs])
            m["selcol"] = f32(np.tile(np.array([[0.0, 1.0]], np.float32), (128, 1)))
            hmask[:, 0:2] = 0.0
        else:
            m["fmask"] = np.stack([ones, tri])
            m["madd"] = np.stack([zeros, cdiag])
            m["selcol"] = f32(np.tile(np.array([[1.0, 0.0]], np.float32), (128, 1)))
        m["hmask"] = hmask
        maps.append(m)
    return maps


_NC_CACHE = {}


def kernel(**inputs):
    x = np.asarray(inputs["x"])
    nb, seq = x.shape[0], x.shape[1]
    configure(seq)
    key = (nb, seq)
    if key not in _NC_CACHE:
        _NC_CACHE[key] = Builder(ncores=2 * nb).build()
    nc = _NC_CACHE[key]
    maps = host_inputs(inputs)
    res = run_bass_kernel_spmd(nc, maps, core_ids=list(range(2 * nb)))
    out = np.empty((nb, seq, D), np.float32)
    for core in range(2 * nb):
        b, r = core // 2, core % 2
        o = np.asarray(res.results[core]["out"], np.float32).reshape(NT, 128, D)
        out[b].reshape(NGT, 128, D)[r::2] = o
    return out
```
